# Optimizing a Trainium2 kernel written in Bass

```python
import jax, jax.numpy as jnp
from jax import lax
import numpy as np

D_MODEL = 1024
BATCH = 16
SEQ = 2048
DEPTH = 4

NSA_HEADS = 8
NSA_KV_GROUPS = 2
NSA_HPG = NSA_HEADS // NSA_KV_GROUPS
NSA_HEAD_DIM = 64
NSA_WIDTH = NSA_HEADS * NSA_HEAD_DIM
NSA_KV_WIDTH = NSA_KV_GROUPS * NSA_HEAD_DIM
NSA_N_KV = 6
CMP_BLOCK = 32
CMP_STRIDE = 16
SEL_BLOCK = 64
SEL_TOP_N = 16
WINDOW = 512
NSA_QBLOCK = 64
POOL_WINDOWS = (2, 4, 8, 16)
POOL_GROUPS = len(POOL_WINDOWS)
POOL_WIDTH = D_MODEL - NSA_WIDTH
POOL_GROUP_DIM = POOL_WIDTH // POOL_GROUPS
A_IN = NSA_WIDTH + NSA_N_KV * NSA_KV_WIDTH + 3 * NSA_HEADS + POOL_WIDTH
GLA_HEADS = 4
GLA_KEY_WIDTH = D_MODEL // 2
GLA_VAL_WIDTH = D_MODEL
GLA_DK = GLA_KEY_WIDTH // GLA_HEADS
GLA_DV = GLA_VAL_WIDTH // GLA_HEADS
GLA_GATE_RANK = 16
GLA_TAU = 16.0
GLA_CHUNK = 64
C_IN = 2 * GLA_KEY_WIDTH + 2 * GLA_VAL_WIDTH + GLA_GATE_RANK
D_FF = 4 * D_MODEL
N_EVEN = (DEPTH + 1) // 2
N_ODD = DEPTH // 2
ALPHA = (2 * DEPTH) ** 0.25
BETA = (8 * DEPTH) ** -0.25
LN_EPS = 1e-5
MASK_VALUE = -1e30
FORCE_SCORE = 1e4

kernel_name = "nsa_pool_gla_deepnorm_hybrid"


def layer_norm(x, g, b):
    xf = x.astype(jnp.float32)
    mu = jnp.mean(xf, axis=-1, keepdims=True)
    var = jnp.mean(jnp.square(xf - mu), axis=-1, keepdims=True)
    return ((xf - mu) * lax.rsqrt(var + LN_EPS) * g + b).astype(x.dtype)


def masked_softmax(s, mask):
    s = jnp.where(mask, s.astype(jnp.float32), MASK_VALUE)
    m = jnp.max(s, axis=-1, keepdims=True)
    e = jnp.where(mask, jnp.exp(s - m), 0.0)
    return e / jnp.maximum(jnp.sum(e, axis=-1, keepdims=True), 1e-30)


def compress_blocks(k, pos, w1, b1, w2):
    S = k.shape[2]
    n_cmp = (S - CMP_BLOCK) // CMP_STRIDE + 1
    idx = np.arange(n_cmp)[:, None] * CMP_STRIDE + np.arange(CMP_BLOCK)[None, :]
    blocks = k[:, :, idx, :] + pos
    flat = blocks.reshape(blocks.shape[:3] + (CMP_BLOCK * NSA_HEAD_DIM,))
    return jax.nn.gelu(flat @ w1 + b1) @ w2


def selection_map(n_cmp, n_sel):
    sub = np.arange(n_cmp)[:, None] + np.arange(CMP_BLOCK // CMP_STRIDE)[None, :]
    owner = sub // (SEL_BLOCK // CMP_STRIDE)
    return (owner[:, :, None] == np.arange(n_sel)[None, None, :]).sum(1).astype(np.float32)


def pool_mixer(u, pool_w, pool_scale):
    B, S, _ = u.shape
    uf = u.astype(jnp.float32).reshape(B, S, POOL_GROUPS, POOL_GROUP_DIM)
    cs = jnp.pad(jnp.cumsum(uf, axis=1), ((0, 0), (1, 0), (0, 0), (0, 0)))
    count_base = jnp.arange(1, S + 1, dtype=jnp.float32)
    outs = []
    for g, w in enumerate(POOL_WINDOWS):
        c = cs[:, :, g]
        lower = jnp.pad(c[:, :S + 1 - w], ((0, 0), (w - 1, 0), (0, 0)))
        mean = (c[:, 1:] - lower) / jnp.minimum(count_base, float(w))[None, :, None]
        outs.append(mean - uf[:, :, g])
    r = jnp.stack(outs, axis=2)
    y = jnp.einsum('bsgc,gcd->bsgd', r, pool_w.astype(jnp.float32))
    return (y.reshape(B, S, POOL_WIDTH) * pool_scale).astype(u.dtype)


def nsa_pool_mixer(x, w_in, cmp_pos, cmp_w1, cmp_b1, cmp_w2, pool_w, pool_scale, w_out):
    B, S, _ = x.shape
    G, HPG, DH = NSA_KV_GROUPS, NSA_HPG, NSA_HEAD_DIM
    h = x @ w_in
    o1 = NSA_WIDTH
    o2 = o1 + NSA_N_KV * NSA_KV_WIDTH
    o3 = o2 + 3 * NSA_HEADS
    q = h[..., :o1].reshape(B, S, G, HPG, DH).transpose(0, 2, 3, 1, 4)
    kv = h[..., o1:o2].reshape(B, S, NSA_N_KV, G, DH).transpose(2, 0, 3, 1, 4)
    gates = jax.nn.sigmoid(h[..., o2:o3].astype(jnp.float32))
    gates = gates.reshape(B, S, G, HPG, 3).transpose(0, 2, 3, 1, 4)
    u = h[..., o3:]
    k_cmp, v_cmp, k_slc, v_slc, k_win, v_win = kv[0], kv[1], kv[2], kv[3], kv[4], kv[5]

    kc = compress_blocks(k_cmp, cmp_pos[0], cmp_w1[0], cmp_b1[0], cmp_w2[0])
    vc = compress_blocks(v_cmp, cmp_pos[1], cmp_w1[1], cmp_b1[1], cmp_w2[1])
    n_cmp = kc.shape[2]
    n_sel = S // SEL_BLOCK
    top_n = min(SEL_TOP_N, n_sel)
    sel_map = jnp.asarray(selection_map(n_cmp, n_sel))
    cmp_end = jnp.arange(n_cmp) * CMP_STRIDE + CMP_BLOCK - 1
    ks_b = k_slc.reshape(B, G, n_sel, SEL_BLOCK, DH)
    vs_b = v_slc.reshape(B, G, n_sel, SEL_BLOCK, DH)
    kw = jnp.pad(k_win, ((0, 0), (0, 0), (WINDOW, 0), (0, 0)))
    vw = jnp.pad(v_win, ((0, 0), (0, 0), (WINDOW, 0), (0, 0)))
    bi = jnp.arange(B)[:, None, None, None]
    gi = jnp.arange(G)[None, :, None, None]
    scale = DH ** -0.5

    def query_block(i):
        t0 = i * NSA_QBLOCK
        t = t0 + jnp.arange(NSA_QBLOCK)
        qb = lax.dynamic_slice_in_dim(q, t0, NSA_QBLOCK, axis=3) * scale
        gb = lax.dynamic_slice_in_dim(gates, t0, NSA_QBLOCK, axis=3)
        s_c = jnp.einsum('bghqd,bgcd->bghqc', qb, kc)
        p_c = masked_softmax(s_c, cmp_end[None, :] <= t[:, None])
        o_c = jnp.einsum('bghqc,bgcd->bghqd', p_c.astype(vc.dtype), vc)
        imp = jnp.einsum('bghqc,cj->bgqj', p_c, sel_map)
        blk = jnp.arange(n_sel)[None, :]
        cur = (t // SEL_BLOCK)[:, None]
        forced = (blk == 0) | (blk == cur) | (blk == cur - 1)
        imp = jnp.where(forced, FORCE_SCORE, jnp.where(blk > cur, -FORCE_SCORE, imp))
        _, sel = lax.top_k(imp, top_n)
        k_sel = ks_b[bi, gi, sel].reshape(B, G, NSA_QBLOCK, top_n * SEL_BLOCK, DH)
        v_sel = vs_b[bi, gi, sel].reshape(B, G, NSA_QBLOCK, top_n * SEL_BLOCK, DH)
        kpos = (sel[..., None] * SEL_BLOCK + jnp.arange(SEL_BLOCK)).reshape(
            B, G, NSA_QBLOCK, top_n * SEL_BLOCK)
        s_s = jnp.einsum('bghqd,bgqnd->bghqn', qb, k_sel)
        p_s = masked_softmax(s_s, (kpos <= t[:, None])[:, :, None])
        o_s = jnp.einsum('bghqn,bgqnd->bghqd', p_s.astype(v_sel.dtype), v_sel)
        k_w = lax.dynamic_slice_in_dim(kw, t0, WINDOW + NSA_QBLOCK, axis=2)
        v_w = lax.dynamic_slice_in_dim(vw, t0, WINDOW + NSA_QBLOCK, axis=2)
        wpos = t0 - WINDOW + jnp.arange(WINDOW + NSA_QBLOCK)
        wmask = ((wpos[None, :] <= t[:, None]) & (wpos[None, :] > t[:, None] - WINDOW)
                 & (wpos[None, :] >= 0))
        s_w = jnp.einsum('bghqd,bgkd->bghqk', qb, k_w)
        p_w = masked_softmax(s_w, wmask)
        o_w = jnp.einsum('bghqk,bgkd->bghqd', p_w.astype(v_w.dtype), v_w)
        o = gb[..., 0:1] * o_c + gb[..., 1:2] * o_s + gb[..., 2:3] * o_w
        return o.astype(x.dtype)

    ob = lax.map(query_block, jnp.arange(S // NSA_QBLOCK))
    o_nsa = ob.transpose(1, 0, 4, 2, 3, 5).reshape(B, S, NSA_WIDTH)
    o_pool = pool_mixer(u, pool_w, pool_scale)
    return jnp.concatenate([o_nsa, o_pool], axis=-1) @ w_out


def gla_mixer(x, w_in, gate_w2, gate_b, norm_g, w_out):
    B, S, _ = x.shape
    N, C, H = S // GLA_CHUNK, GLA_CHUNK, GLA_HEADS
    h = x @ w_in
    o1 = GLA_KEY_WIDTH
    o2 = o1 + GLA_KEY_WIDTH
    o3 = o2 + GLA_VAL_WIDTH
    o4 = o3 + GLA_VAL_WIDTH
    q_, k_, v_, r, a = h[..., :o1], h[..., o1:o2], h[..., o2:o3], h[..., o3:o4], h[..., o4:]
    loga = jax.nn.log_sigmoid((a @ gate_w2 + gate_b).astype(jnp.float32)) / GLA_TAU

    def heads(t, d):
        return t.reshape(B, N, C, H, d).transpose(0, 3, 1, 2, 4).astype(jnp.float32)

    q = heads(q_, GLA_DK) * GLA_DK ** -0.5
    k = heads(k_, GLA_DK)
    v = heads(v_, GLA_DV)
    b = jnp.cumsum(heads(loga, GLA_DK), axis=3)
    q_t = q * jnp.exp(b)
    k_t = k * jnp.exp(-b)
    causal = jnp.tril(jnp.ones((C, C), dtype=bool))
    att = jnp.where(causal, jnp.einsum('bhncd,bhnjd->bhncj', q_t, k_t), 0.0)
    o_intra = jnp.einsum('bhncj,bhnje->bhnce', att, v)
    b_last = b[:, :, :, -1:, :]
    upd = jnp.einsum('bhncd,bhnce->bhnde', k * jnp.exp(b_last - b), v)
    decay = jnp.exp(b_last[:, :, :, 0])

    def step(state, inp):
        d, u = inp
        return d[..., None] * state + u, state

    s0 = jnp.zeros((B, H, GLA_DK, GLA_DV), jnp.float32)
    _, s_prev = lax.scan(step, s0, (jnp.moveaxis(decay, 2, 0), jnp.moveaxis(upd, 2, 0)))
    s_prev = jnp.moveaxis(s_prev, 0, 2)
    o = o_intra + jnp.einsum('bhncd,bhnde->bhnce', q_t, s_prev)
    o = o * lax.rsqrt(jnp.mean(jnp.square(o), axis=-1, keepdims=True) + LN_EPS) * norm_g
    o = o.transpose(0, 2, 3, 1, 4).reshape(B, S, GLA_VAL_WIDTH).astype(x.dtype)
    return (o * jax.nn.silu(r)) @ w_out


def sqrelu_mlp(x, w1, w2):
    return jnp.square(jax.nn.relu(x @ w1)) @ w2


def setup_inputs(seed: int = 0) -> dict:
    key = jax.random.key(seed)
    ks = jax.random.split(key, 24)

    def nrm(k, shape, scale):
        return jax.random.normal(k, shape, jnp.float32) * scale

    DH = NSA_HEAD_DIM
    return {
        "x": nrm(ks[0], (BATCH, SEQ, D_MODEL), 1.0),
        "a_w_in": nrm(ks[1], (N_EVEN, D_MODEL, A_IN), D_MODEL ** -0.5),
        "a_cmp_pos": nrm(ks[2], (N_EVEN, 2, CMP_BLOCK, DH), 0.1),
        "a_cmp_w1": nrm(ks[3], (N_EVEN, 2, CMP_BLOCK * DH, DH), (CMP_BLOCK * DH) ** -0.5),
        "a_cmp_b1": nrm(ks[4], (N_EVEN, 2, DH), 0.02),
        "a_cmp_w2": nrm(ks[5], (N_EVEN, 2, DH, DH), DH ** -0.5),
        "a_pool_w": nrm(ks[6], (N_EVEN, POOL_GROUPS, POOL_GROUP_DIM, POOL_GROUP_DIM), POOL_GROUP_DIM ** -0.5),
        "a_pool_scale": 1.0 + nrm(ks[7], (N_EVEN, POOL_WIDTH), 0.1),
        "a_w_out": nrm(ks[8], (N_EVEN, D_MODEL, D_MODEL), BETA * D_MODEL ** -0.5),
        "c_w_in": nrm(ks[9], (N_ODD, D_MODEL, C_IN), D_MODEL ** -0.5),
        "c_gate_w2": nrm(ks[10], (N_ODD, GLA_GATE_RANK, GLA_KEY_WIDTH), GLA_GATE_RANK ** -0.5),
        "c_gate_b": nrm(ks[11], (N_ODD, GLA_KEY_WIDTH), 0.1),
        "c_norm_g": 1.0 + nrm(ks[12], (N_ODD, GLA_DV), 0.1),
        "c_w_out": nrm(ks[13], (N_ODD, D_MODEL, D_MODEL), BETA * D_MODEL ** -0.5),
        "ln1_g": 1.0 + nrm(ks[14], (DEPTH, D_MODEL), 0.05),
        "ln1_b": nrm(ks[15], (DEPTH, D_MODEL), 0.02),
        "ln2_g": 1.0 + nrm(ks[16], (DEPTH, D_MODEL), 0.05),
        "ln2_b": nrm(ks[17], (DEPTH, D_MODEL), 0.02),
        "mlp_w1": nrm(ks[18], (DEPTH, D_MODEL, D_FF), D_MODEL ** -0.5),
        "mlp_w2": nrm(ks[19], (DEPTH, D_FF, D_MODEL), BETA * D_FF ** -0.5),
    }


def reference(x, a_w_in, a_cmp_pos, a_cmp_w1, a_cmp_b1, a_cmp_w2, a_pool_w, a_pool_scale,
              a_w_out, c_w_in, c_gate_w2, c_gate_b, c_norm_g, c_w_out,
              ln1_g, ln1_b, ln2_g, ln2_b, mlp_w1, mlp_w2):
    for i in range(DEPTH):
        j = i // 2
        if i % 2 == 0:
            mix = nsa_pool_mixer(x, a_w_in[j], a_cmp_pos[j], a_cmp_w1[j], a_cmp_b1[j],
                                 a_cmp_w2[j], a_pool_w[j], a_pool_scale[j], a_w_out[j])
        else:
            mix = gla_mixer(x, c_w_in[j], c_gate_w2[j], c_gate_b[j], c_norm_g[j], c_w_out[j])
        x = layer_norm(ALPHA * x + mix, ln1_g[i], ln1_b[i])
        x = layer_norm(ALPHA * x + sqrelu_mlp(x, mlp_w1[i], mlp_w2[i]), ln2_g[i], ln2_b[i])
    return x
```

```python
import numpy as np
import ml_dtypes
from contextlib import ExitStack
import concourse.bass as bass
import concourse.mybir as mybir
from concourse.bass_utils import run_bass_kernel_spmd

F32 = mybir.dt.float32
BF16 = mybir.dt.bfloat16
ALU = mybir.AluOpType
AF = mybir.ActivationFunctionType
AX = mybir.AxisListType

S = 2048
D = 1024
NT = 16
DEPTH = 4
ALPHA = float((2 * DEPTH) ** 0.25)
LN_EPS = 1e-5
NEG = -30000.0
A_IN = 1816
C_IN = 3088


class R:
    __slots__ = ("w", "rs")

    def __init__(self):
        self.w = None
        self.rs = {}


class Ctx:
    NDS = 40

    def __init__(self, nc, es):
        self.nc = nc
        self.es = es
        self.E = {"pe": nc.tensor, "dve": nc.vector, "act": nc.scalar, "pool": nc.gpsimd, "sp": nc.sync}
        self.sem = {k: es.enter_context(nc.semaphore("c_" + k)) for k in ("pe", "dve", "act", "pool")}
        self.cnt = {k: 0 for k in self.sem}
        self.seen = {k: {} for k in self.E}
        self.dsem = [es.enter_context(nc.semaphore("d%d" % i)) for i in range(self.NDS)]
        self.dcnt = [0] * self.NDS
        self.dtok = [None] * self.NDS
        self.dnext = 0
        self.nins = 0

    def sb(self, es, name, shape, dt=F32):
        self.nsb = getattr(self, "nsb", 0) + 1
        return es.enter_context(self.nc.sbuf_tensor("%s_%d" % (name, self.nsb), list(shape), dt))

    def _wait(self, eng, tok):
        teng, sem, val = tok
        sn = sem.name
        if self.seen[eng].get(sn, 0) >= val:
            return
        self.E[eng].wait_ge(sem, val)
        self.seen[eng][sn] = val
        self.nins += 1

    def _deps(self, eng, r, w, is_dma):
        for x in r:
            tok = x.w
            if tok is not None:
                if (not is_dma) and tok[0] == eng and eng == "pe":
                    continue
                self._wait(eng, tok)
        for x in w:
            tok = x.w
            if tok is not None and (is_dma or tok[0] != eng):
                self._wait(eng, tok)
            for t in x.rs.values():
                if is_dma or t[0] != eng:
                    self._wait(eng, t)

    def _mark(self, tok, r, w):
        sn = tok[1].name
        for x in r:
            x.rs[sn] = tok
        for x in w:
            x.w = tok
            x.rs = {}

    def op(self, eng, fn, r=(), w=()):
        self._deps(eng, r, w, False)
        ins = fn(self.E[eng])
        ins.then_inc(self.sem[eng], 1)
        self.cnt[eng] += 1
        tok = (eng, self.sem[eng], self.cnt[eng])
        self._mark(tok, r, w)
        self.nins += 1
        return tok

    def dma(self, q, out, in_, r=(), w=(), **kw):
        i = self.dnext
        self.dnext = (self.dnext + 1) % self.NDS
        if self.dtok[i] is not None:
            self._wait(q, self.dtok[i])
        self._deps(q, r, w, True)
        ins = self.E[q].dma_start(out=out, in_=in_, **kw)
        ins.then_inc(self.dsem[i], 16)
        self.dcnt[i] += 16
        tok = ("dma", self.dsem[i], self.dcnt[i])
        self.dtok[i] = tok
        self._mark(tok, r, w)
        self.nins += 1
        return tok

    def barrier(self):
        toks = [(k, self.sem[k], self.cnt[k]) for k in self.sem if self.cnt[k] > 0]
        toks += [t for t in self.dtok if t is not None]
        for e in self.E:
            for t in toks:
                if t[0] != e:
                    self._wait(e, t)

    def finish(self):
        for t in self.dtok:
            if t is not None:
                self._wait("sp", t)


class Ring:
    def __init__(self, c, es, name, shape, dt, n):
        self.bufs = [c.sb(es, "%s%d" % (name, i), shape, dt) for i in range(n)]
        self.rs = [R() for _ in range(n)]
        self.i = 0

    def get(self):
        k = self.i
        self.i = (self.i + 1) % len(self.bufs)
        return self.bufs[k], self.rs[k]


def _consts():
    r = np.arange(128)[:, None]
    k = np.arange(128)[None, :]
    cs = {}
    cs["c_identf"] = np.eye(128, dtype=np.float32)
    cs["c_identb"] = np.eye(128, dtype=np.float32).astype(ml_dtypes.bfloat16)
    cs["c_causal"] = np.where(k <= r, 0.0, NEG).astype(np.float32)
    kk = np.arange(640)[None, :]
    cs["c_winb"] = np.where((kk > r) & (kk <= r + 512), 0.0, NEG).astype(np.float32).astype(ml_dtypes.bfloat16)
    jj = np.arange(247)[None, :]
    cs["c_cmp"] = np.where(16 * (jj - 120) + 31 - r <= 0, 0.0, NEG).astype(np.float32)
    selA = np.zeros((128, 16, 32), np.float32)
    selB = np.zeros((128, 16, 32), np.float32)
    blk = np.arange(32)[None, :]
    for qt in range(16):
        cur = 2 * qt + (np.arange(128)[:, None] >= 64)
        forced = (blk == 0) | (blk == cur) | (blk == cur - 1)
        fut = blk > cur
        selA[:, qt, :] = np.where(forced | fut, 0.0, 1.0)
        selB[:, qt, :] = np.where(forced, 1e4, np.where(fut, -1e4, 0.0))
    cs["c_selA"] = selA
    cs["c_selB"] = selB
    n_cmp, n_sel = 127, 32
    sub = np.arange(n_cmp)[:, None] + np.arange(2)[None, :]
    owner = sub // 4
    sm = (owner[:, :, None] == np.arange(n_sel)[None, None, :]).sum(1).astype(np.float32)
    selmap = np.zeros((128, 32), np.float32)
    selmap[:127] = sm
    cs["c_selmap"] = selmap
    cs["c_tri"] = np.where(r <= k, -1.0 / 16.0, 0.0).astype(np.float32)
    cs["c_ut"] = np.where(r <= k, 1.0, 0.0).astype(np.float32)
    band = np.zeros((128, 12, 128), np.float32)
    j = np.arange(128)[:, None]
    t = np.arange(128)[None, :]
    for g, w in enumerate((2, 4, 8, 16)):
        band[:, 0 * 4 + g, :] = np.where((j <= t) & (j > t - w), 1.0 / w, 0.0) - (j == t)
        band[:, 1 * 4 + g, :] = np.where((j - 128 > t - w), 1.0 / w, 0.0)
        cnt = np.minimum(t + 1, w)
        band[:, 2 * 4 + g, :] = np.where((j <= t) & (j > t - w), 1.0 / cnt, 0.0) - (j == t)
    cs["c_band"] = band.astype(ml_dtypes.bfloat16)
    return cs


CONST_SHAPES = {"c_identf": ([128, 128], F32), "c_identb": ([128, 128], BF16), "c_causal": ([128, 128], F32),
                "c_winb": ([128, 640], BF16), "c_cmp": ([128, 247], F32), "c_selA": ([128, 16, 32], F32),
                "c_selB": ([128, 16, 32], F32), "c_selmap": ([128, 32], F32), "c_tri": ([128, 128], F32),
                "c_ut": ([128, 128], F32), "c_band": ([128, 12, 128], BF16)}

W_SHAPES = {
    "a_w_in": [2, 1024, A_IN], "a_cmp_pos": [2, 2, 32, 64], "a_cmp_w1": [2, 2, 2048, 64], "a_cmp_b1": [2, 2, 64],
    "a_cmp_w2": [2, 2, 64, 64], "a_pool_w": [2, 4, 128, 128], "a_pool_scale": [2, 512], "a_w_out": [2, 1024, 1024],
    "c_w_in": [2, 1024, C_IN], "c_gate_w2": [2, 16, 512], "c_gate_b": [2, 512], "c_norm_g": [2, 256],
    "c_w_out": [2, 1024, 1024], "ln1_g": [4, 1024], "ln1_b": [4, 1024], "ln2_g": [4, 1024], "ln2_b": [4, 1024],
    "mlp_w1": [4, 1024, 4096], "mlp_w2": [4, 4096, 1024],
}


def _a_perm():
    cols = []
    for p in range(4):
        cols += list(range(p * 64, p * 64 + 64)) + list(range((4 + p) * 64, (4 + p) * 64 + 64))
    for n in (0, 1, 2, 4):
        cols += list(range(512 + n * 128, 512 + n * 128 + 128))
    for n in (3, 5):
        cols += list(range(512 + n * 128, 512 + n * 128 + 128))
    cols += list(range(1304, 1816))
    cols += list(range(1280, 1304))
    return np.array(cols)


def build(nseq=2, nlayers=DEPTH, dbg=False):
    nc = bass.Bass("TRN2", target_bir_lowering=False)
    x_d = nc.dram_tensor("x", [nseq, S, D], F32, kind="ExternalInput").ap()
    out_d = nc.dram_tensor("out", [nseq, S, D], F32, kind="ExternalOutput").ap()
    dbg_d = nc.dram_tensor("dbg", [2 * nlayers, S, D], F32, kind="ExternalOutput").ap() if dbg else None
    Wd = {k: nc.dram_tensor(k, v, F32, kind="ExternalInput").ap() for k, v in W_SHAPES.items()}
    Cd = {k: nc.dram_tensor(k, v[0], v[1], kind="ExternalInput").ap() for k, v in CONST_SHAPES.items()}
    w1b_d = nc.dram_tensor("w1b", [DEPTH, 1024, 4096], BF16).ap()
    w2b_d = nc.dram_tensor("w2b", [DEPTH, 4096, 1024], BF16).ap()

    with ExitStack() as es:
        c = Ctx(nc, es)
        RX = [R() for _ in range(NT)]
        xres = Ring(c, es, "xres", [128, D], F32, 2)
        pend = {}

        def xpre(t, src):
            if t not in pend and t < NT:
                buf, rr = xres.get()
                c.dma("sp", buf[:], src[t * 128:(t + 1) * 128, :], r=[RX[t]], w=[rr])
                pend[t] = (buf, rr)
        XT = c.sb(es, "XT", [128, 8, S], BF16)
        RXT = [R() for _ in range(NT)]
        PS = es.enter_context(nc.psum_tensor("PS", [128, 6, 512], F32))
        PSf = PS[:].rearrange("p a b -> p (a b)")
        RB = [R() for _ in range(6)]
        PB = es.enter_context(nc.psum_tensor("PB", [128, 2, 1024], BF16))
        RPB = [R() for _ in range(2)]
        st = {"pb": 0, "ptr": 0}

        def alloc(n):
            if st["ptr"] + n > 5:
                st["ptr"] = 0
            b = st["ptr"]
            st["ptr"] = (st["ptr"] + n) % 5
            return b

        def allocb():
            b = st["pb"]
            st["pb"] ^= 1
            return b

        K = {}
        Rc = R()
        for k, (shp, dt) in CONST_SHAPES.items():
            K[k] = c.sb(es, "s" + k, shp, dt)
            c.dma("sp", K[k][:], Cd[k], w=[Rc])
        identb, identf = K["c_identb"], K["c_identf"]
        LNP = [c.sb(es, "lnp%d" % i, [128, D], F32) for i in range(2)]
        RLN = R()
        WR = Ring(c, es, "wr", [128, 4096], BF16, 3)

        class WQ:
            q = []
            nxt = 0
            slots = {}

        def wpush(views):
            WQ.q.extend(views)

        def wget():
            i = WQ.nxt
            lim = min(len(WQ.q), i + 2)
            for jn in range(i, lim):
                if jn not in WQ.slots:
                    buf, rr = WR.get()
                    src, Rsrc = WQ.q[jn]
                    shp = src.shape
                    dst = buf[:, 0:shp[1] * shp[2]].rearrange("p (a b) -> p a b", b=shp[2])
                    c.dma("pool", dst, src, r=([Rsrc] if Rsrc is not None else []), w=[rr])
                    WQ.slots[jn] = (dst, rr)
            WQ.nxt += 1
            return WQ.slots.pop(i)

        def kview(ap2d, c0, c1):
            return ap2d.rearrange("(k p) n -> p k n", p=128)[:, :, c0:c1]

        RWB = [R() for _ in range(DEPTH)]
        conv_q = []

        def conv_push(l):
            for k in range(8):
                conv_q.append((w1b_d[l, k * 128:(k + 1) * 128, :], Wd["mlp_w1"][l, k * 128:(k + 1) * 128, :], RWB[l]))
            for k in range(8):
                conv_q.append((w2b_d[l, k * 512:(k + 1) * 512, :].rearrange("(p a) n -> p a n", p=128),
                               Wd["mlp_w2"][l, k * 512:(k + 1) * 512, :].rearrange("(p a) n -> p a n", p=128), RWB[l]))

        def conv_step(n=1):
            for _ in range(n):
                if not conv_q:
                    return
                dst, src, rr = conv_q.pop(0)
                c.dma("pool", dst, src, w=[rr])

        def layer_slabs(l):
            v = []
            jj = l // 2
            if l % 2 == 0:
                w = Wd["a_w_in"][jj]
                v += [kview(w, 0, 512), kview(w, 512, 1024), kview(w, 1024, 1536), kview(w, 1536, 1816)]
                w = Wd["a_w_out"][jj]
                v += [kview(w, 0, 512), kview(w, 512, 1024)]
            else:
                w = Wd["c_w_in"][jj]
                for tb in range(4):
                    v += [kview(w, i * 512, (i + 1) * 512) for i in range(6)] + [kview(w, 3072, 3088)]
                w = Wd["c_w_out"][jj]
                v += [kview(w, 0, 512), kview(w, 512, 1024)]
            v = [(x_, None) for x_ in v]
            return v

        lnXb = Ring(c, es, "lnXb", [128, D], BF16, 6)
        deferred = []

        def flush_def(n=1):
            for _ in range(n):
                if not deferred:
                    return
                deferred.pop(0)()

        lnS = Ring(c, es, "lnS", [128, 16], F32, 2)

        def to_xt(t, src_b, Rsrc):
            pb = allocb()
            for k in range(8):
                c.op("pe", lambda e: e.transpose(PB[:, pb, k * 128:(k + 1) * 128], src_b[:, k * 128:(k + 1) * 128], identb[:]),
                     r=[Rsrc, Rc], w=[RPB[pb]])
            c.op("act", lambda e: e.copy(XT[:, :, t * 128:(t + 1) * 128],
                                         PB[:, pb, :].rearrange("p (a b) -> p a b", b=128)), w=[RPB[pb], RXT[t]])

        def ln_tile(t, b, xsrc, xdst, dbg_i=None, s=0, last=False, pre=None):
            gam, bet = LNP
            sv, Rs = lnS.get()
            if pre is not None:
                Y, RY = pre
            else:
                xpre(t, xsrc)
                Y, RY = pend.pop(t)
                c.op("dve", lambda e: e.scalar_tensor_tensor(out=Y[:], in0=Y[:], scalar=ALPHA, in1=PSf[:, b * 512:b * 512 + 1024],
                                                             op0=ALU.mult, op1=ALU.add), r=[RY], w=[RY, RB[b], RB[b + 1]])
            c.op("dve", lambda e: e.bn_stats(out=sv[:, 0:6], in_=Y[:, 0:512]), r=[RY], w=[Rs])
            c.op("dve", lambda e: e.bn_stats(out=sv[:, 6:12], in_=Y[:, 512:1024]), r=[RY], w=[Rs])
            c.op("dve", lambda e: e.bn_aggr(out=sv[:, 12:14], in_=sv[:, 0:12]), r=[Rs], w=[Rs])
            c.op("act", lambda e: e.activation(out=sv[:, 14:15], in_=sv[:, 13:14], func=AF.Ln, bias=LN_EPS, scale=1.0), r=[Rs], w=[Rs])
            c.op("act", lambda e: e.activation(out=sv[:, 14:15], in_=sv[:, 14:15], func=AF.Exp, scale=-0.5), r=[Rs], w=[Rs])
            c.op("dve", lambda e: e.scalar_tensor_tensor(out=sv[:, 15:16], in0=sv[:, 12:13], scalar=-1.0, in1=sv[:, 14:15],
                                                         op0=ALU.mult, op1=ALU.mult), r=[Rs], w=[Rs])
            c.op("act", lambda e: e.activation(out=Y[:], in_=Y[:], func=AF.Identity, bias=sv[:, 15:16], scale=sv[:, 14:15]),
                 r=[RY, Rs], w=[RY])
            c.op("dve", lambda e: e.tensor_tensor(out=Y[:], in0=Y[:], in1=gam[:], op=ALU.mult), r=[RLN, RY], w=[RY])
            c.op("dve", lambda e: e.tensor_tensor(out=Y[:], in0=Y[:], in1=bet[:], op=ALU.add), r=[RLN, RY], w=[RY])
            c.dma("pool", xdst[t * 128:(t + 1) * 128, :], Y[:], r=[RY], w=[RX[t]])
            if dbg_i is not None and dbg and s == 0:
                c.dma("sp", dbg_d[dbg_i, t * 128:(t + 1) * 128, :], Y[:], r=[RY])
            if last:
                return
            xb, Rxb = lnXb.get()
            c.op("act", lambda e: e.copy(xb[:], Y[:]), r=[RY], w=[Rxb])
            deferred.append(lambda t=t, xb=xb, Rxb=Rxb: to_xt(t, xb, Rxb))

        def load_ln(l, which):
            g = Wd["ln%d_g" % which][l]
            bta = Wd["ln%d_b" % which][l]
            c.dma("sp", LNP[0][:], g.partition_broadcast(128), w=[RLN])
            c.dma("sp", LNP[1][:], bta.partition_broadcast(128), w=[RLN])

        def mlp(l, s, last):
            with ExitStack() as ms:
                HT = c.sb(ms, "HT", [128, 32, 512], BF16)
                RH = R()
                rl = Ring(c, ms, "relu", [128, 512], F32, 2)
                Yr = Ring(c, ms, "Yr", [128, D], F32, 5)
                conv_step(99)
                load_ln(l, 2)
                WRm = Ring(c, ms, "wrm", [128, 4096], BF16, 6)
                mslabs = []
                for tb_ in range(4):
                    mslabs += [kview(w1b_d[l], i * 512, (i + 1) * 512) for i in range(8)]
                    for hf_ in range(2):
                        mslabs += [w2b_d[l][i * 512:(i + 1) * 512, hf_ * 512:(hf_ + 1) * 512].rearrange("(k p) n -> p k n", p=128)
                                   for i in range(8)]
                mq = {"nxt": 0, "slots": {}}

                def mget():
                    i0 = mq["nxt"]
                    for jn in range(i0, min(len(mslabs), i0 + 5)):
                        if jn not in mq["slots"]:
                            buf, rr = WRm.get()
                            src = mslabs[jn]
                            shp = src.shape
                            dst = buf[:, 0:shp[1] * shp[2]].rearrange("p (a b) -> p a b", b=shp[2])
                            c.dma("sp", dst, src, r=[RWB[l]], w=[rr])
                            mq["slots"][jn] = (dst, rr)
                    mq["nxt"] += 1
                    return mq["slots"].pop(i0)

                for tb in range(4):
                    for i in range(8):
                        wv, Rw = mget()
                        for ci in range(4):
                            b = alloc(1)
                            for k in range(8):
                                c.op("pe", lambda e: e.matmul(PS[:, b, :], lhsT=wv[:, k, ci * 128:(ci + 1) * 128],
                                                              rhs=XT[:, k, tb * 512:(tb + 1) * 512], start=(k == 0), stop=(k == 7)),
                                     r=[Rw] + RXT[tb * 4:(tb + 1) * 4], w=[RB[b]])
                            rb, Rr = rl.get()
                            c.op("act", lambda e: e.activation(out=rb[:], in_=PS[:, b, :], func=AF.Relu), w=[RB[b], Rr])
                            c.op("dve", lambda e: e.tensor_tensor(out=HT[:, i * 4 + ci, :], in0=rb[:], in1=rb[:], op=ALU.mult),
                                 r=[Rr], w=[RH])
                        if i >= 1:
                            flush_def(1)
                    ys = []
                    for tt in range(4):
                        t = tb * 4 + tt
                        ybuf, yr = Yr.get()
                        c.dma("sp", ybuf[:], out_d[s][t * 128:(t + 1) * 128, :], r=[RX[t]], w=[yr])
                        ys.append((ybuf, yr))
                    for hf in range(2):
                        b = alloc(4)
                        for i in range(8):
                            wv, Rw = mget()
                            for tt in range(4):
                                for k in range(4):
                                    f = i * 4 + k
                                    c.op("pe", lambda e: e.matmul(PS[:, b + tt, :], lhsT=HT[:, f, tt * 128:(tt + 1) * 128],
                                                                  rhs=wv[:, k, :], start=(f == 0), stop=(f == 31)),
                                         r=[Rw, RH], w=[RB[b + tt]])
                        for tt in range(4):
                            Y, RY = ys[tt]
                            c.op("dve", lambda e: e.scalar_tensor_tensor(out=Y[:, hf * 512:(hf + 1) * 512], in0=Y[:, hf * 512:(hf + 1) * 512],
                                                                         scalar=ALPHA, in1=PS[:, b + tt, :], op0=ALU.mult, op1=ALU.add),
                                 r=[RY], w=[RY, RB[b + tt]])
                    for tt in range(4):
                        t = tb * 4 + tt
                        ln_tile(t, None, None, out_d[s], dbg_i=2 * l + 1, s=s, last=last, pre=ys[tt])
                flush_def(99)
            c.barrier()

        def wout_ln(l, s):
            xsrc = x_d[s] if l == 0 else out_d[s]
            (w0, R0) = wget()
            (w1_, R1) = wget()
            xpre(0, xsrc)
            for t in range(NT):
                xpre(t + 1, xsrc)
                b = alloc(2)
                for hf, (wv, Rw) in enumerate(((w0, R0), (w1_, R1))):
                    for k in range(8):
                        c.op("pe", lambda e: e.matmul(PS[:, b + hf, :], lhsT=XT[:, k, t * 128:(t + 1) * 128], rhs=wv[:, k, :],
                                                      start=(k == 0), stop=(k == 7)), r=[Rw, RXT[t]], w=[RB[b + hf]])
                if len(deferred) >= 2:
                    flush_def(1)
                ln_tile(t, b, xsrc, out_d[s], dbg_i=2 * l, s=s)
            flush_def(99)

        def even_mixer(l, s):
            j = l // 2
            with ExitStack() as ms:
                QT = c.sb(ms, "QT", [128, 4, S], BF16); RQT = R()
                KT2 = c.sb(ms, "KT2", [128, 2, S], BF16); RKT = R()
                V = c.sb(ms, "V", [128, NT, 256], BF16); RV = R()
                G = c.sb(ms, "G", [128, NT, 24], F32); RG = R()
                KCT = c.sb(ms, "KCT", [128, 128], BF16); RKC = R()
                VC = c.sb(ms, "VC", [128, 2, 64], F32); RVC = R()
                load_ln(l, 1)
                with ExitStack() as ps1:
                    KC2 = c.sb(ps1, "KC2", [128, 2, S], BF16); RKC2 = R()
                    U = c.sb(ps1, "U", [128, NT, 512], BF16); RU = R()
                    W1c = c.sb(ps1, "W1c", [128, 2, 32, 64], BF16); RW1 = R()
                    W2k = c.sb(ps1, "W2k", [64, 128], BF16)
                    W2v = c.sb(ps1, "W2v", [64, 64], F32)
                    posN = c.sb(ps1, "posN", [32, 2, 64], F32)
                    posT = c.sb(ps1, "posT", [64, 2, 32], BF16); RposT = R()
                    b1 = c.sb(ps1, "b1", [64, 2], F32)
                    PW = c.sb(ps1, "PW", [128, 4, 128], BF16)
                    PSC = c.sb(ps1, "PSC", [128, 4], F32)
                    for kv in range(2):
                        src = Wd["a_cmp_w1"][j, kv].rearrange("(l d) o -> d l o", d=64)
                        c.dma("pool", W1c[0:64, kv], src, w=[RW1])
                        c.dma("pool", W1c[64:128, kv], src, w=[RW1])
                    c.dma("sp", posN[:], Wd["a_cmp_pos"][j].rearrange("k l d -> l k d"), w=[RW1])
                    c.dma("pool", W2k[:, 0:64], Wd["a_cmp_w2"][j, 0], w=[RW1])
                    c.dma("pool", W2k[:, 64:128], Wd["a_cmp_w2"][j, 0], w=[RW1])
                    c.dma("sp", W2v[:], Wd["a_cmp_w2"][j, 1], w=[RW1])
                    c.dma("sp", b1[:], Wd["a_cmp_b1"][j].rearrange("k d -> d k"), w=[RW1], allow_slow_non_contiguous=True)
                    c.dma("pool", PW[:], Wd["a_pool_w"][j].rearrange("g c d -> c g d"), w=[RW1])
                    c.dma("sp", PSC[:], Wd["a_pool_scale"][j].rearrange("(g d) -> d g", d=128), w=[RW1], allow_slow_non_contiguous=True)

                    for sl in range(2):
                        wv, Rw = wget()
                        for ci in range(4):
                            if sl == 0:
                                dest, Rd = QT[:, ci, :], RQT
                            elif ci < 2:
                                dest, Rd = KC2[:, ci, :], RKC2
                            else:
                                dest, Rd = KT2[:, ci - 2, :], RKT
                            for tb in range(4):
                                b = alloc(1)
                                for k in range(8):
                                    c.op("pe", lambda e: e.matmul(PS[:, b, :], lhsT=wv[:, k, ci * 128:(ci + 1) * 128],
                                                                  rhs=XT[:, k, tb * 512:(tb + 1) * 512], start=(k == 0), stop=(k == 7)),
                                         r=[Rw] + RXT[tb * 4:(tb + 1) * 4], w=[RB[b]])
                                c.op("act", lambda e: e.activation(out=dest[:, tb * 512:(tb + 1) * 512], in_=PS[:, b, :], func=AF.Copy,
                                                                   scale=(0.125 if sl == 0 else 1.0)), w=[RB[b], Rd])
                    (wa, Ra) = wget()
                    (wb, Rb_) = wget()
                    for t in range(NT):
                        b = alloc(2)
                        for k in range(8):
                            c.op("pe", lambda e: e.matmul(PS[:, b, :], lhsT=XT[:, k, t * 128:(t + 1) * 128], rhs=wa[:, k, :],
                                                          start=(k == 0), stop=(k == 7)), r=[Ra, RXT[t]], w=[RB[b]])
                        for k in range(8):
                            c.op("pe", lambda e: e.matmul(PS[:, b + 1, 0:280], lhsT=XT[:, k, t * 128:(t + 1) * 128], rhs=wb[:, k, :],
                                                          start=(k == 0), stop=(k == 7)), r=[Rb_, RXT[t]], w=[RB[b + 1]])
                        c.op("act", lambda e: e.copy(V[:, t, :], PS[:, b, 0:256]), w=[RB[b], RV])
                        c.op("dve", lambda e: e.tensor_copy(U[:, t, 0:256], PS[:, b, 256:512]), w=[RB[b], RU])
                        c.op("dve", lambda e: e.tensor_copy(U[:, t, 256:512], PS[:, b + 1, 0:256]), w=[RB[b + 1], RU])
                        c.op("act", lambda e: e.activation(out=G[:, t, :], in_=PS[:, b + 1, 256:280], func=AF.Sigmoid), w=[RB[b + 1], RG])

                    band = K["c_band"]
                    rT = Ring(c, ps1, "rT", [128, 512], BF16, 3)
                    def pool_tail(g, tb, rt_, Rrt):
                        b2 = alloc(1)
                        c.op("pe", lambda e: e.matmul(PS[:, b2, :], lhsT=PW[:, g, :], rhs=rt_[:], start=True, stop=True),
                             r=[Rrt, RW1], w=[RB[b2]])
                        c.op("dve", lambda e: e.tensor_scalar(XT[:, 4 + g, tb * 512:(tb + 1) * 512], PS[:, b2, :], PSC[:, g:g + 1], None,
                                                              op0=ALU.mult), r=[RW1], w=[RB[b2]] + RXT[tb * 4:(tb + 1) * 4])

                    prev_pool = None
                    for g in range(4):
                        for tb in range(4):
                            b = alloc(1)
                            for ti in range(4):
                                t = tb * 4 + ti
                                o_ = PS[:, b, ti * 128:(ti + 1) * 128]
                                if t == 0:
                                    c.op("pe", lambda e: e.matmul(o_, lhsT=U[:, 0, g * 128:(g + 1) * 128], rhs=band[:, 8 + g, :],
                                                                  start=True, stop=True), r=[RU, Rc], w=[RB[b]])
                                else:
                                    c.op("pe", lambda e: e.matmul(o_, lhsT=U[:, t, g * 128:(g + 1) * 128], rhs=band[:, g, :],
                                                                  start=True, stop=False), r=[RU, Rc], w=[RB[b]])
                                    c.op("pe", lambda e: e.matmul(o_, lhsT=U[:, t - 1, g * 128:(g + 1) * 128], rhs=band[:, 4 + g, :],
                                                                  start=False, stop=True), r=[RU, Rc], w=[RB[b]])
                            rt_, Rrt = rT.get()
                            c.op("act", lambda e: e.copy(rt_[:], PS[:, b, :]), w=[RB[b], Rrt])
                            if prev_pool is not None:
                                pool_tail(*prev_pool)
                            prev_pool = (g, tb, rt_, Rrt)
                    pool_tail(*prev_pool)

                    cw = Ring(c, ps1, "cw", [64, 128], F32, 4)
                    cg = Ring(c, ps1, "cg", [64, 128], BF16, 2)
                    cgf = Ring(c, ps1, "cgf", [64, 128], F32, 2)
                    bias = c.sb(ps1, "cbias", [64, 2], F32); Rbias = R()
                    for kv in range(2):
                        b = alloc(1)
                        c.op("pe", lambda e: e.matmul(PS[0:64, b, 0:32], lhsT=posN[0:32, kv, :], rhs=identf[0:32, 0:32], start=True, stop=True),
                             r=[RW1, Rc], w=[RB[b]])
                        c.op("act", lambda e: e.copy(posT[:, kv, :], PS[0:64, b, 0:32]), w=[RB[b], RposT])
                    for kv in range(2):
                        b = alloc(1)
                        for l_ in range(32):
                            c.op("pe", lambda e: e.matmul(PS[0:64, b, 0:1], lhsT=W1c[0:64, kv, l_, :], rhs=posT[:, kv, l_:l_ + 1],
                                                          start=(l_ == 0), stop=(l_ == 31)), r=[RW1, RposT], w=[RB[b]])
                        c.op("dve", lambda e: e.tensor_tensor(out=bias[:, kv:kv + 1], in0=PS[0:64, b, 0:1], in1=b1[:, kv:kv + 1], op=ALU.add),
                             r=[RW1], w=[RB[b], Rbias])
                    for kv in range(2):
                        for g in range(2):
                            pr = slice(g * 64, (g + 1) * 64)
                            b = alloc(1)
                            for l_ in range(32):
                                c.op("pe", lambda e: e.matmul(PS[0:64, b, 0:127], lhsT=W1c[pr, kv, l_, :],
                                                              rhs=KC2[pr, kv, l_:l_ + 16 * 126 + 1:16], start=(l_ == 0), stop=(l_ == 31)),
                                     r=[RW1, RKC2], w=[RB[b]])
                            z, Rz = cw.get()
                            c.op("act", lambda e: e.activation(out=z[:, 0:127], in_=PS[0:64, b, 0:127], func=AF.Identity,
                                                               bias=bias[:, kv:kv + 1], scale=1.0), r=[Rbias], w=[RB[b], Rz])
                            t1, Rt1 = cw.get()
                            c.op("dve", lambda e: e.tensor_tensor(out=t1[:, 0:127], in0=z[:, 0:127], in1=z[:, 0:127], op=ALU.mult),
                                 r=[Rz], w=[Rt1])
                            c.op("dve", lambda e: e.tensor_scalar(t1[:, 0:127], t1[:, 0:127], 0.044715, 1.0, op0=ALU.mult, op1=ALU.add),
                                 r=[Rt1], w=[Rt1])
                            c.op("dve", lambda e: e.tensor_tensor(out=t1[:, 0:127], in0=t1[:, 0:127], in1=z[:, 0:127], op=ALU.mult),
                                 r=[Rz, Rt1], w=[Rt1])
                            c.op("act", lambda e: e.activation(out=t1[:, 0:127], in_=t1[:, 0:127], func=AF.Sigmoid, scale=1.5957691216057308),
                                 r=[Rt1], w=[Rt1])
                            if kv == 0:
                                ge, Rge = cg.get()
                            else:
                                ge, Rge = cgf.get()
                            c.op("dve", lambda e: e.tensor_tensor(out=ge[:, 0:127], in0=t1[:, 0:127], in1=z[:, 0:127], op=ALU.mult),
                                 r=[Rz, Rt1], w=[Rge])
                            b2 = alloc(1)
                            if kv == 0:
                                c.op("pe", lambda e: e.matmul(PS[:, b2, 0:127], lhsT=W2k[:, :], rhs=ge[:, 0:127], start=True, stop=True),
                                     r=[Rge, RW1], w=[RB[b2]])
                                c.op("act", lambda e: e.copy(KCT[pr, 0:127], PS[pr, b2, 0:127]), w=[RB[b2], RKC])
                            else:
                                c.op("pe", lambda e: e.matmul(PS[0:127, b2, 0:64], lhsT=ge[:, 0:127], rhs=W2v[:, :], start=True, stop=True),
                                     r=[Rge, RW1], w=[RB[b2]])
                                c.op("act", lambda e: e.copy(VC[0:127, g, :], PS[0:127, b2, 0:64]), w=[RB[b2], RVC])
                c.barrier()

                P_ = Ring(c, ms, "P", [128, 2048], BF16, 4)
                PT = Ring(c, ms, "PT", [128, 2048], BF16, 3)
                MF = Ring(c, ms, "MF", [128, 2048], BF16, 4)
                sm4 = Ring(c, ms, "sm4", [128, 8], F32, 10)
                scr = Ring(c, ms, "scr", [128, 1024], F32, 1)
                ppr = Ring(c, ms, "ppr", [128, 1024], F32, 1)
                ptr_ = Ring(c, ms, "ptr", [128, 1024], F32, 1)
                str_ = Ring(c, ms, "str", [128, 24], F32, 2)
                Or = Ring(c, ms, "O", [128, 512], F32, 3)
                Ob = Ring(c, ms, "Ob", [128, 512], BF16, 2)
                tk = Ring(c, ms, "tk", [128, 4, 64], F32, 2)
                m8r = Ring(c, ms, "m8", [128, 32], F32, 2)
                winb = K["c_winb"]
                for zi in range(1):
                    c.op("pool", lambda e: e.memset(ppr.bufs[zi][:], 0.0), w=[ppr.rs[zi]])

                def v3(ap):
                    return ap.rearrange("p (a b) -> p a b", b=128)

                QS = {}

                def cmp1(qt):
                    qs = slice(qt * 128, (qt + 1) * 128)
                    O, RO = Or.get()
                    bc = alloc(2)
                    for h in range(8):
                        g, hh = divmod(h, 4)
                        pr = slice(g * 64, (g + 1) * 64)
                        c.op("pe", lambda e: e.matmul(PSf[:, bc * 512 + h * 128:bc * 512 + h * 128 + 127], lhsT=QT[pr, hh, qs],
                                                      rhs=KCT[pr, 0:127], start=True, stop=True), r=[RQT, RKC], w=[RB[bc + h // 4]])
                    sc, Rsc = scr.get()
                    st_, Rst = str_.get()
                    sc3 = v3(sc[:])[:, :, 0:127]
                    c.op("dve", lambda e: e.tensor_tensor(out=sc3, in0=v3(PSf[:, bc * 512:bc * 512 + 1024])[:, :, 0:127],
                                                          in1=K["c_cmp"][:, 120 - 8 * qt:247 - 8 * qt].unsqueeze(1).to_broadcast([128, 8, 127]),
                                                          op=ALU.add), r=[Rc], w=[Rsc, RB[bc], RB[bc + 1]])
                    c.op("dve", lambda e: e.tensor_reduce(out=st_[:, 0:8], in_=sc3, axis=AX.X, op=ALU.max), r=[Rsc], w=[Rst])
                    c.op("dve", lambda e: e.tensor_scalar(st_[:, 0:8], st_[:, 0:8], -10000.0, None, op0=ALU.max), r=[Rst], w=[Rst])
                    c.op("dve", lambda e: e.tensor_tensor(out=sc3, in0=sc3, in1=st_[:, 0:8].unsqueeze(2).to_broadcast([128, 8, 127]),
                                                          op=ALU.subtract), r=[Rsc, Rst], w=[Rsc])
                    c.op("act", lambda e: e.activation(out=sc3, in_=sc3, func=AF.Exp), r=[Rsc], w=[Rsc])
                    c.op("dve", lambda e: e.tensor_reduce(out=st_[:, 8:16], in_=sc3, axis=AX.X, op=ALU.add), r=[Rsc], w=[Rst])
                    c.op("dve", lambda e: e.tensor_scalar(st_[:, 8:16], st_[:, 8:16], 1e-30, None, op0=ALU.max), r=[Rst], w=[Rst])
                    c.op("dve", lambda e: e.reciprocal(st_[:, 16:24], st_[:, 8:16]), r=[Rst], w=[Rst])
                    pp, Rpp = ppr.get()
                    c.op("dve", lambda e: e.tensor_tensor(out=v3(pp[:])[:, :, 0:127], in0=sc3,
                                                          in1=st_[:, 16:24].unsqueeze(2).to_broadcast([128, 8, 127]), op=ALU.mult),
                         r=[Rsc, Rst], w=[Rpp])
                    QS[qt] = dict(O=O, RO=RO, pp=pp, Rpp=Rpp, qs=qs)

                def cmp2(qt):
                    pp, Rpp = QS[qt]['pp'], QS[qt]['Rpp']
                    bt = alloc(2)
                    for h in range(8):
                        c.op("pe", lambda e: e.matmul(PSf[:, bt * 512 + h * 128:bt * 512 + (h + 1) * 128], lhsT=pp[:, h * 128:(h + 1) * 128],
                                                      rhs=identf[:, :], start=True, stop=True), r=[Rpp, Rc], w=[RB[bt + h // 4]])
                    ptc, Rptc = ptr_.get()
                    c.op("act", lambda e: e.copy(ptc[:], PSf[:, bt * 512:bt * 512 + 1024]), w=[RB[bt], RB[bt + 1], Rptc])
                    QS[qt].update(ptc=ptc, Rptc=Rptc)

                def cmp3(qt):
                    ptc, Rptc, O, RO = QS[qt]['ptc'], QS[qt]['Rptc'], QS[qt]['O'], QS[qt]['RO']
                    for g in range(2):
                        for hh in range(4):
                            h = g * 4 + hh
                            c.op("pe", lambda e: e.matmul(PS[:, 5, g * 32:(g + 1) * 32], lhsT=ptc[0:127, h * 128:(h + 1) * 128],
                                                          rhs=K["c_selmap"][0:127, :], start=(hh == 0), stop=(hh == 3)),
                                 r=[Rptc, Rc], w=[RB[5]])
                    bo = alloc(1)
                    for h in range(8):
                        c.op("pe", lambda e: e.matmul(PS[:, bo, h * 64:(h + 1) * 64], lhsT=ptc[0:127, h * 128:(h + 1) * 128],
                                                      rhs=VC[0:127, h // 4, :], start=True, stop=True), r=[Rptc, RVC], w=[RB[bo]])
                    c.op("dve", lambda e: e.tensor_tensor(out=O[:].rearrange("p (h d) -> p h d", d=64),
                                                          in0=PS[:, bo, :].rearrange("p (h d) -> p h d", d=64),
                                                          in1=G[:, qt, :].rearrange("p (h i) -> p h i", i=3)[:, :, 0:1].to_broadcast([128, 8, 64]),
                                                          op=ALU.mult), r=[RG], w=[RB[bo], RO])

                def cmp4(qt):
                    mfs = []
                    if qt >= 8:
                        tkb, Rtk = tk.get()
                        m8, Rm8 = m8r.get()
                        c.op("dve", lambda e: e.tensor_tensor(out=tkb[:, 0, :].rearrange("p (g j) -> p g j", j=32),
                                                              in0=PS[:, 5, 0:64].rearrange("p (g j) -> p g j", j=32),
                                                              in1=K["c_selA"][:, qt, :].unsqueeze(1).to_broadcast([128, 2, 32]), op=ALU.mult),
                             r=[Rc], w=[RB[5], Rtk])
                        c.op("dve", lambda e: e.tensor_tensor(out=tkb[:, 0, :].rearrange("p (g j) -> p g j", j=32),
                                                              in0=tkb[:, 0, :].rearrange("p (g j) -> p g j", j=32),
                                                              in1=K["c_selB"][:, qt, :].unsqueeze(1).to_broadcast([128, 2, 32]), op=ALU.add),
                             r=[Rc, Rtk], w=[Rtk])
                        for g in range(2):
                            gs = slice(g * 32, (g + 1) * 32)
                            c.op("dve", lambda e: e.max(out=m8[:, g * 16:g * 16 + 8], in_=tkb[:, 0, gs]), r=[Rtk], w=[Rm8])
                            c.op("dve", lambda e: e.match_replace(out=tkb[:, 1, gs], in_to_replace=m8[:, g * 16:g * 16 + 8],
                                                                  in_values=tkb[:, 0, gs], imm_value=-3e4), r=[Rtk, Rm8], w=[Rtk])
                            c.op("dve", lambda e: e.max(out=m8[:, g * 16 + 8:g * 16 + 16], in_=tkb[:, 1, gs]), r=[Rtk], w=[Rm8])
                            c.op("dve", lambda e: e.tensor_scalar(tkb[:, 2, gs], tkb[:, 0, gs], m8[:, g * 16 + 15:g * 16 + 16], None,
                                                                  op0=ALU.is_ge), r=[Rtk, Rm8], w=[Rtk])
                        c.op("dve", lambda e: e.tensor_scalar(tkb[:, 3, :], tkb[:, 2, :], -NEG, NEG, op0=ALU.mult, op1=ALU.add),
                             r=[Rtk], w=[Rtk])
                    for g in range(2):
                        mf, Rmf = MF.get()
                        if qt >= 8:
                            c.op("pool", lambda e: e.tensor_copy(mf[:, 0:qt * 128].rearrange("p (j k) -> p j k", k=64),
                                                                 tkb[:, 3, g * 32:g * 32 + 2 * qt].unsqueeze(2).to_broadcast([128, 2 * qt, 64])),
                                 r=[Rtk], w=[Rmf])
                        elif qt > 0:
                            c.op("pool", lambda e: e.memset(mf[:, 0:qt * 128], 0.0), w=[Rmf])
                        c.op("pool", lambda e: e.tensor_copy(mf[:, qt * 128:(qt + 1) * 128], K["c_causal"][:]), r=[Rc], w=[Rmf])
                        mfs.append((mf, Rmf))
                    QS[qt]['mfs'] = mfs

                def run_items():
                    items = [(qt_, g, hh, br) for qt_ in range(NT) for g in range(2) for hh in range(4) for br in range(2)]
                    stt = {}

                    def stA(it):
                        qt, g, hh, br = it
                        qs = QS[qt]['qs']
                        mfs = QS[qt]['mfs']
                        pr = slice(g * 64, (g + 1) * 64)
                        if br == 0:
                            kt0 = 0
                            mt, Rm = mfs[g]
                            moff = 0
                        else:
                            kt0 = max(0, qt - 4)
                            mt, Rm = winb, Rc
                            moff = 640 - (qt - kt0 + 1) * 128
                        L = (qt - kt0 + 1) * 128
                        nb = (L + 511) // 512
                        b = alloc(nb)
                        for ci in range(nb):
                            n0 = ci * 512
                            n1 = min(L, n0 + 512)
                            o_ = PSf[:, b * 512 + n0:b * 512 + n1]
                            c.op("pe", lambda e: e.matmul(o_, lhsT=QT[pr, hh, qs], rhs=KT2[pr, br, kt0 * 128 + n0:kt0 * 128 + n1],
                                                          start=True, stop=False), r=[RQT, RKT], w=[RB[b + ci]])
                            c.op("pe", lambda e: e.matmul(o_, lhsT=identb[:, :], rhs=mt[:, moff + n0:moff + n1], start=False, stop=True),
                                 r=[Rc, Rm], w=[RB[b + ci]])
                        stt[it] = dict(b=b, nb=nb, L=L, kt0=kt0)

                    def stB(it):
                        qt, g, hh, br = it
                        d_ = stt[it]
                        b, nb, L = d_["b"], d_["nb"], d_["L"]
                        h = g * 4 + hh
                        ps_ap = PSf[:, b * 512:b * 512 + L]
                        banks = [RB[b + ci] for ci in range(nb)]
                        s4, Rs4 = sm4.get()
                        c.op("dve", lambda e: e.tensor_reduce(out=s4[:, 0:1], in_=ps_ap, axis=AX.X, op=ALU.max), w=[Rs4] + banks)
                        c.op("dve", lambda e: e.tensor_scalar(s4[:, 1:2], s4[:, 0:1], -1.0, None, op0=ALU.mult), r=[Rs4], w=[Rs4])
                        p, Rp = P_.get()
                        c.op("act", lambda e: e.activation(out=p[:, 0:L], in_=ps_ap, func=AF.Exp, bias=s4[:, 1:2], scale=1.0,
                                                           accum_out=s4[:, 2:3]), r=[Rs4], w=[Rp, Rs4] + banks)
                        c.op("dve", lambda e: e.reciprocal(s4[:, 4:5], s4[:, 2:3]), r=[Rs4], w=[Rs4])
                        gcol = h * 3 + 1 + br
                        c.op("dve", lambda e: e.tensor_tensor(out=s4[:, 3:4], in0=G[:, qt, gcol:gcol + 1], in1=s4[:, 4:5], op=ALU.mult),
                             r=[RG, Rs4], w=[Rs4])
                        d_.update(p=p, Rp=Rp, s4=s4, Rs4=Rs4)

                    def stC(it):
                        d_ = stt[it]
                        L, p, Rp = d_["L"], d_["p"], d_["Rp"]
                        nk = L // 128
                        pt, Rpt = PT.get()
                        for k0 in range(0, nk, 8):
                            n = min(8, nk - k0)
                            pb = allocb()
                            for kk in range(n):
                                c.op("pe", lambda e: e.transpose(PB[:, pb, kk * 128:(kk + 1) * 128], p[:, (k0 + kk) * 128:(k0 + kk + 1) * 128],
                                                                 identb[:]), r=[Rp, Rc], w=[RPB[pb]])
                            c.op("act", lambda e: e.copy(pt[:, k0 * 128:(k0 + n) * 128], PB[:, pb, 0:n * 128]), w=[RPB[pb], Rpt])
                        d_.update(pt=pt, Rpt=Rpt)

                    def stD(it):
                        qt, g, hh, br = it
                        O, RO = QS[qt]['O'], QS[qt]['RO']
                        d_ = stt[it]
                        L, kt0, pt, Rpt, s4, Rs4 = d_["L"], d_["kt0"], d_["pt"], d_["Rpt"], d_["s4"], d_["Rs4"]
                        nk = L // 128
                        h = g * 4 + hh
                        vcol = br * 128 + g * 64
                        bo_ = alloc(1)
                        for kk in range(nk):
                            c.op("pe", lambda e: e.matmul(PS[:, bo_, 0:64], lhsT=pt[:, kk * 128:(kk + 1) * 128], rhs=V[:, kt0 + kk, vcol:vcol + 64],
                                                          start=(kk == 0), stop=(kk == nk - 1)), r=[Rpt, RV], w=[RB[bo_]])
                        c.op("dve", lambda e: e.scalar_tensor_tensor(out=O[:, h * 64:(h + 1) * 64], in0=PS[:, bo_, 0:64], scalar=s4[:, 3:4],
                                                                     in1=O[:, h * 64:(h + 1) * 64], op0=ALU.mult, op1=ALU.add),
                             r=[Rs4], w=[RB[bo_], RO])

                    def fin(qt):
                        O, RO, qs = QS[qt]['O'], QS[qt]['RO'], QS[qt]['qs']
                        ob, Rob = Ob.get()
                        c.op("act", lambda e: e.copy(ob[:], O[:]), r=[RO], w=[Rob])
                        pb = allocb()
                        for k in range(4):
                            c.op("pe", lambda e: e.transpose(PB[:, pb, k * 128:(k + 1) * 128], ob[:, k * 128:(k + 1) * 128], identb[:]),
                                 r=[Rob, Rc], w=[RPB[pb]])
                        c.op("act", lambda e: e.copy(XT[:, 0:4, qs], PB[:, pb, 0:512].rearrange("p (a b) -> p a b", b=128)),
                             w=[RPB[pb], RXT[qt]])
                        QS.pop(qt)

                    n_it = len(items)
                    for i in range(n_it + 5):
                        if i < n_it:
                            qt_i, off = divmod(i, 16)
                            if off == 0:
                                conv_step(1)
                            if qt_i + 1 < NT:
                                if off == 1:
                                    cmp1(qt_i + 1)
                                elif off == 5:
                                    cmp2(qt_i + 1)
                                elif off == 8:
                                    cmp3(qt_i + 1)
                                elif off == 11:
                                    cmp4(qt_i + 1)
                            stA(items[i])
                            stB(items[i])
                        if 2 <= i < n_it + 2:
                            stC(items[i - 2])
                        if 3 <= i < n_it + 3:
                            stD(items[i - 3])
                        if i >= 5 and (i - 5) % 16 == 15:
                            fin((i - 5) // 16)

                cmp1(0)
                cmp2(0)
                cmp3(0)
                cmp4(0)
                run_items()
            c.barrier()
            wout_ln(l, s)

        def odd_mixer(l, s):
            j = l // 2
            with ExitStack() as ms:
                gw2 = c.sb(ms, "gw2", [32, 512], F32); Rgw = R()
                NG = c.sb(ms, "NG", [128, 4, 256], F32)
                Sst = c.sb(ms, "Sst", [128, 4, 256], F32); RS = R()
                Sbf = c.sb(ms, "Sbf", [128, 4, 256], BF16); RSb = R()
                QR = c.sb(ms, "QR", [128, 4, 512], BF16); RQR = R()
                KR = c.sb(ms, "KR", [128, 4, 512], BF16); RKR = R()
                Vb = c.sb(ms, "Vb", [128, 4, 1024], BF16); RVb = R()
                SRb = c.sb(ms, "SRb", [128, 4, 1024], BF16); RSR = R()
                AT = c.sb(ms, "AT", [32, 512], F32); RAT = R()
                c.dma("sp", gw2[0:16, :], Wd["c_gate_w2"][j], w=[Rgw])
                c.dma("sp", gw2[16:17, :], Wd["c_gate_b"][j].rearrange("(o n) -> o n", o=1), w=[Rgw])
                for hd in range(4):
                    c.dma("sp", NG[:, hd, :], Wd["c_norm_g"][j].partition_broadcast(128), w=[Rgw])
                load_ln(l, 1)
                c.op("pool", lambda e: e.memset(Sst[:], 0.0), w=[RS])
                c.op("pool", lambda e: e.memset(Sbf[:], 0.0), w=[RSb])
                c.op("pool", lambda e: e.memset(AT[:], 1.0), w=[RAT])
                f512 = Ring(c, ms, "f512", [128, 512], F32, 14)
                b512 = Ring(c, ms, "b512", [128, 512], BF16, 18)
                og = Ring(c, ms, "og", [128, 1024], BF16, 4)
                tu = Ring(c, ms, "tu", [128, 256], F32, 17)
                junk = c.sb(ms, "junk", [128, 256], F32); Rjunk = R()
                ssr = Ring(c, ms, "ssr", [128, 8], F32, 2)
                tri, ut = K["c_tri"], K["c_ut"]
                NGf = NG[:].rearrange("p a b -> p (a b)")
                for tb in range(4):
                    bs = slice(tb * 512, (tb + 1) * 512)
                    for which, (dst, Rd) in enumerate(((QR, RQR), (KR, RKR))):
                        wv, Rw = wget()
                        for ci in range(4):
                            b = alloc(1)
                            for k in range(8):
                                c.op("pe", lambda e: e.matmul(PS[:, b, :], lhsT=wv[:, k, ci * 128:(ci + 1) * 128], rhs=XT[:, k, bs],
                                                              start=(k == 0), stop=(k == 7)), r=[Rw] + RXT[tb * 4:(tb + 1) * 4], w=[RB[b]])
                            c.op("act", lambda e: e.copy(dst[:, ci, :], PS[:, b, :]), w=[RB[b], Rd])
                    for hf in range(2):
                        wv, Rw = wget()
                        for ti in range(4):
                            t = tb * 4 + ti
                            b = alloc(1)
                            for k in range(8):
                                c.op("pe", lambda e: e.matmul(PS[:, b, :], lhsT=XT[:, k, t * 128:(t + 1) * 128], rhs=wv[:, k, :],
                                                              start=(k == 0), stop=(k == 7)), r=[Rw, RXT[t]], w=[RB[b]])
                            c.op("act", lambda e: e.copy(Vb[:, ti, hf * 512:(hf + 1) * 512], PS[:, b, :]), w=[RB[b], RVb])
                    for hf in range(2):
                        wv, Rw = wget()
                        for ti in range(4):
                            t = tb * 4 + ti
                            b = alloc(1)
                            for k in range(8):
                                c.op("pe", lambda e: e.matmul(PS[:, b, :], lhsT=XT[:, k, t * 128:(t + 1) * 128], rhs=wv[:, k, :],
                                                              start=(k == 0), stop=(k == 7)), r=[Rw, RXT[t]], w=[RB[b]])
                            sl_, Rsl = f512.get()
                            c.op("act", lambda e: e.activation(out=sl_[:], in_=PS[:, b, :], func=AF.Silu), w=[RB[b], Rsl])
                            c.op("pool", lambda e: e.tensor_tensor(out=SRb[:, ti, hf * 512:(hf + 1) * 512], in0=sl_[:],
                                                                   in1=NGf[:, hf * 512:(hf + 1) * 512], op=ALU.mult), r=[Rgw, Rsl], w=[RSR])
                    wv, Rw = wget()
                    b = alloc(1)
                    for k in range(8):
                        c.op("pe", lambda e: e.matmul(PS[0:16, b, :], lhsT=wv[:, k, :], rhs=XT[:, k, bs], start=(k == 0), stop=(k == 7)),
                             r=[Rw] + RXT[tb * 4:(tb + 1) * 4], w=[RB[b]])
                    c.op("act", lambda e: e.copy(AT[0:16, :], PS[0:16, b, :]), w=[RB[b], RAT])
                    FS = {}

                    def f1(ti):
                        t = tb * 4 + ti
                        cs_ = slice(ti * 128, (ti + 1) * 128)
                        bz = alloc(1)
                        c.op("pe", lambda e: e.matmul(PS[:, bz, :], lhsT=AT[0:17, cs_], rhs=gw2[0:17, :], start=True, stop=True),
                             r=[RAT, Rgw], w=[RB[bz]])
                        l1, Rl1 = f512.get()
                        c.op("act", lambda e: e.activation(out=l1[:], in_=PS[:, bz, :], func=AF.Exp, scale=-1.0), w=[RB[bz], Rl1])
                        c.op("act", lambda e: e.activation(out=l1[:], in_=l1[:], func=AF.Ln, bias=1.0, scale=1.0), r=[Rl1], w=[Rl1])
                        FS[ti] = dict(t=t, ti=ti, l1=l1, Rl1=Rl1)

                    def f2(ti):
                        t = tb * 4 + ti
                        cs_ = slice(ti * 128, (ti + 1) * 128)
                        l1, Rl1 = FS[ti]['l1'], FS[ti]['Rl1']
                        bb = alloc(1)
                        for hd in range(4):
                            c.op("pe", lambda e: e.matmul(PS[:, bb, hd * 128:(hd + 1) * 128], lhsT=l1[:, hd * 128:(hd + 1) * 128], rhs=tri[:, :],
                                                          start=True, stop=True), r=[Rl1, Rc], w=[RB[bb]])
                        eb, Reb = f512.get()
                        enb, Renb = f512.get()
                        c.op("act", lambda e: e.activation(out=eb[:], in_=PS[:, bb, :], func=AF.Exp), w=[RB[bb], Reb])
                        c.op("act", lambda e: e.activation(out=enb[:], in_=PS[:, bb, :], func=AF.Exp, scale=-1.0), w=[RB[bb], Renb])
                        qtT, Rq = b512.get()
                        ktT, Rk = b512.get()
                        c.op("dve", lambda e: e.scalar_tensor_tensor(out=qtT[:].rearrange("p (a b) -> p a b", b=128), in0=QR[:, :, cs_],
                                                                     scalar=float(128 ** -0.5), in1=eb[:].rearrange("p (a b) -> p a b", b=128),
                                                                     op0=ALU.mult, op1=ALU.mult), r=[Reb, RQR], w=[Rq])
                        c.op("dve", lambda e: e.tensor_tensor(out=ktT[:].rearrange("p (a b) -> p a b", b=128), in0=KR[:, :, cs_],
                                                              in1=enb[:].rearrange("p (a b) -> p a b", b=128), op=ALU.mult),
                             r=[Renb, RKR], w=[Rk])
                        FS[ti].update(eb=eb, Reb=Reb, qtT=qtT, Rq=Rq, ktT=ktT, Rk=Rk)

                    def f3(ti):
                        t = tb * 4 + ti
                        cs_ = slice(ti * 128, (ti + 1) * 128)
                        qtT, Rq, ktT, Rk = FS[ti]['qtT'], FS[ti]['Rq'], FS[ti]['ktT'], FS[ti]['Rk']
                        kt_, Rkt = b512.get()
                        pb = allocb()
                        for hd in range(4):
                            c.op("pe", lambda e: e.transpose(PB[:, pb, hd * 128:(hd + 1) * 128], ktT[:, hd * 128:(hd + 1) * 128], identb[:]),
                                 r=[Rk, Rc], w=[RPB[pb]])
                        c.op("act", lambda e: e.copy(kt_[:], PB[:, pb, 0:512]), w=[RPB[pb], Rkt])
                        batt = alloc(1)
                        for hd in range(4):
                            hs = slice(hd * 128, (hd + 1) * 128)
                            c.op("pe", lambda e: e.matmul(PS[:, batt, hs], lhsT=ktT[:, hs], rhs=qtT[:, hs], start=True, stop=True),
                                 r=[Rq, Rk], w=[RB[batt]])
                        am, Ram = b512.get()
                        c.op("dve", lambda e: e.tensor_tensor(out=am[:].rearrange("p (a b) -> p a b", b=128),
                                                              in0=PS[:, batt, :].rearrange("p (a b) -> p a b", b=128),
                                                              in1=ut[:].unsqueeze(1).to_broadcast([128, 4, 128]), op=ALU.mult),
                             r=[Rc], w=[RB[batt], Ram])
                        FS[ti].update(kt_=kt_, Rkt=Rkt, am=am, Ram=Ram)

                    def f4(ti):
                        t = tb * 4 + ti
                        cs_ = slice(ti * 128, (ti + 1) * 128)
                        eb, Reb, kt_, Rkt = FS[ti]['eb'], FS[ti]['Reb'], FS[ti]['kt_'], FS[ti]['Rkt']
                        tmps = []
                        if t < NT - 1:
                            bu = alloc(2)
                            for hd in range(4):
                                hs = slice(hd * 128, (hd + 1) * 128)
                                u_ = PSf[:, bu * 512 + hd * 256:bu * 512 + (hd + 1) * 256]
                                c.op("pe", lambda e: e.matmul(u_, lhsT=kt_[:, hs], rhs=Vb[:, ti, hd * 256:(hd + 1) * 256], start=True, stop=True),
                                     r=[Rkt, RVb], w=[RB[bu + hd // 2]])
                            for hd in range(4):
                                u_ = PSf[:, bu * 512 + hd * 256:bu * 512 + (hd + 1) * 256]
                                dec = eb[:, hd * 128 + 127:hd * 128 + 128]
                                tmp, Rtmp = tu.get()
                                c.op("act", lambda e: e.activation(out=tmp[:], in_=u_, func=AF.Identity, scale=dec), r=[Reb],
                                     w=[RB[bu + hd // 2], Rtmp])
                                tmps.append((tmp, Rtmp))
                        FS[ti].update(tmps=tmps)

                    def back(f):
                        t, ti, eb, Reb, qtT, Rq, am, Ram, tmps = (f[k] for k in ("t", "ti", "eb", "Reb", "qtT", "Rq", "am", "Ram", "tmps"))
                        bo = alloc(2)
                        for hd in range(4):
                            hs = slice(hd * 128, (hd + 1) * 128)
                            o_ = PSf[:, bo * 512 + hd * 256:bo * 512 + (hd + 1) * 256]
                            rb_ = RB[bo + hd // 2]
                            c.op("pe", lambda e: e.matmul(o_, lhsT=am[:, hs], rhs=Vb[:, ti, hd * 256:(hd + 1) * 256], start=True, stop=False),
                                 r=[Ram, RVb], w=[rb_])
                            c.op("pe", lambda e: e.matmul(o_, lhsT=qtT[:, hs], rhs=Sbf[:, hd, :], start=False, stop=True),
                                 r=[Rq, RSb], w=[rb_])
                        for hd, (tmp, Rtmp) in enumerate(tmps):
                            dec = eb[:, hd * 128 + 127:hd * 128 + 128]
                            c.op("dve", lambda e: e.scalar_tensor_tensor(out=Sst[:, hd, :], in0=Sst[:, hd, :], scalar=dec, in1=tmp[:],
                                                                         op0=ALU.mult, op1=ALU.add), r=[Reb, Rtmp], w=[RS])
                            c.op("act", lambda e: e.copy(Sbf[:, hd, :], Sst[:, hd, :]), r=[RS], w=[RSb])
                        ss_, Rss = ssr.get()
                        for hd in range(4):
                            o_ = PSf[:, bo * 512 + hd * 256:bo * 512 + (hd + 1) * 256]
                            c.op("act", lambda e: e.activation(out=junk[:], in_=o_, func=AF.Square, accum_out=ss_[:, hd:hd + 1]),
                                 w=[RB[bo + hd // 2], Rjunk, Rss])
                        c.op("act", lambda e: e.activation(out=ss_[:, 4:8], in_=ss_[:, 0:4], func=AF.Ln, bias=LN_EPS, scale=1.0 / 256.0),
                             r=[Rss], w=[Rss])
                        c.op("act", lambda e: e.activation(out=ss_[:, 4:8], in_=ss_[:, 4:8], func=AF.Exp, scale=-0.5), r=[Rss], w=[Rss])
                        og_, Rog = og.get()
                        for hd in range(4):
                            o_ = PSf[:, bo * 512 + hd * 256:bo * 512 + (hd + 1) * 256]
                            c.op("dve", lambda e: e.scalar_tensor_tensor(out=og_[:, hd * 256:(hd + 1) * 256], in0=o_, scalar=ss_[:, 4 + hd:5 + hd],
                                                                         in1=SRb[:, ti, hd * 256:(hd + 1) * 256], op0=ALU.mult, op1=ALU.mult),
                                 r=[Rss, RSR], w=[RB[bo + hd // 2], Rog])
                        flush_def(1)
                        deferred.append(lambda t=t, og_=og_, Rog=Rog: to_xt(t, og_, Rog))

                    for u in range(8):
                        conv_step(1) if u < 4 else None
                        if u < 4:
                            f1(u)
                        if 0 <= u - 1 < 4:
                            f2(u - 1)
                        if 0 <= u - 2 < 4:
                            f3(u - 2)
                        if 0 <= u - 3 < 4:
                            f4(u - 3)
                        if 0 <= u - 4 < 4:
                            back(FS[u - 4])
                flush_def(99)
            c.barrier()
            wout_ln(l, s)

        for s in range(nseq):
            for t in range(NT):
                xpre(t, x_d[s])
                xf, Rxf = pend.pop(t)
                xb, Rxb = lnXb.get()
                c.op("act", lambda e: e.copy(xb[:], xf[:]), r=[Rxf], w=[Rxb])
                to_xt(t, xb, Rxb)
            for l in range(nlayers):
                wpush(layer_slabs(l))
                if s == 0:
                    conv_push(l)
                if l % 2 == 0:
                    even_mixer(l, s)
                else:
                    odd_mixer(l, s)
                mlp(l, s, last=(l == nlayers - 1))
        c.finish()
        print("instructions emitted:", c.nins)
    return nc


def prep_weights(inputs):
    w = {}
    for k in W_SHAPES:
        a = np.ascontiguousarray(np.asarray(inputs[k], dtype=np.float32))
        if k == "a_w_in":
            a = np.ascontiguousarray(a[:, :, _a_perm()])
        w[k] = a
    w.update(_consts())
    return w


_NC_CACHE = {}


def kernel(**inputs):
    x = np.asarray(inputs["x"], dtype=np.float32)
    n = 8
    if "nc" not in _NC_CACHE:
        _NC_CACHE["nc"] = build()
    nc = _NC_CACHE["nc"]
    w = prep_weights(inputs)
    in_maps = []
    for cid in range(n):
        m = dict(w)
        m["x"] = np.ascontiguousarray(x[2 * cid:2 * cid + 2])
        in_maps.append(m)
    res = run_bass_kernel_spmd(nc, in_maps, core_ids=list(range(n)))
    return np.concatenate([r["out"] for r in res.results], axis=0).astype(np.float32)
```
